# Optimizing a Trainium2 kernel written in Bass

```python
import jax, jax.numpy as jnp
from jax import lax
import numpy as np

D_MODEL = 2048
BATCH = 4
SEQ = 8192
DEPTH = 2

HEAD_DIM = 128
N_HEADS_SB = 8
N_HEADS_FOX = 8
N_HEADS_DSA = 8
N_KV_DSA = 2
IDX_HEADS = 16
IDX_DIM = 64
TOPK_MAX = 256
Q_BLOCK = 128
ROPE_THETA = 10000.0
NORM_EPS = 1e-6
N_BRANCH = 3
BRANCH_WIDTH = N_HEADS_SB * HEAD_DIM
SB_W = N_HEADS_SB * HEAD_DIM
FOX_W = N_HEADS_FOX * HEAD_DIM
DSA_W = N_HEADS_DSA * HEAD_DIM
DSA_KV_W = N_KV_DSA * HEAD_DIM
D_FF = -(-8 * D_MODEL // (3 * 256)) * 256
IN_SIZES = (SB_W, SB_W, SB_W,
            FOX_W, FOX_W, FOX_W, N_HEADS_FOX,
            DSA_W, DSA_KV_W, DSA_KV_W,
            IDX_HEADS * IDX_DIM, IDX_DIM, IDX_HEADS)
D_IN = sum(IN_SIZES)

kernel_name = "hybrid_sb_fox_dsa_gated_block"


def rmsnorm(x, g):
    xf = x.astype(jnp.float32)
    y = xf * lax.rsqrt(jnp.mean(xf * xf, axis=-1, keepdims=True) + NORM_EPS)
    return (y * g.astype(jnp.float32)).astype(x.dtype)


def rope_tables(seq, dim):
    inv = 1.0 / (ROPE_THETA ** (jnp.arange(0, dim, 2, dtype=jnp.float32) / dim))
    ang = jnp.arange(seq, dtype=jnp.float32)[:, None] * inv[None, :]
    return jnp.cos(ang), jnp.sin(ang)


def apply_rope(x, cos, sin):
    xf = x.astype(jnp.float32)
    x1, x2 = jnp.split(xf, 2, axis=-1)
    c = cos[None, :, None, :]
    s = sin[None, :, None, :]
    return jnp.concatenate([x1 * c - x2 * s, x2 * c + x1 * s], axis=-1).astype(x.dtype)


def to_blocks(a):
    b, s = a.shape[0], a.shape[1]
    return jnp.moveaxis(a.reshape((b, s // Q_BLOCK, Q_BLOCK) + a.shape[2:]), 1, 0)


def from_blocks(a):
    a = jnp.moveaxis(a, 0, 1)
    return a.reshape((a.shape[0], a.shape[1] * a.shape[2]) + a.shape[3:])


def block_starts(seq):
    return jnp.arange(seq // Q_BLOCK, dtype=jnp.int32) * Q_BLOCK


def stick_breaking_attention(q, k, v):
    S, d = q.shape[1], q.shape[3]
    scale = d ** -0.5
    spos = jnp.arange(S, dtype=jnp.int32)

    def block(args):
        qb, t0 = args
        tpos = t0 + jnp.arange(Q_BLOCK, dtype=jnp.int32)
        strict = (spos[None, :] < tpos[:, None])[None, None]
        z = jnp.einsum('bqhd,bshd->bhqs', qb, k).astype(jnp.float32) * scale
        log_keep = jnp.where(strict, jax.nn.log_sigmoid(-z), 0.0)
        log_after = lax.cumsum(log_keep, axis=3, reverse=True) - log_keep
        a = jnp.where(strict, jnp.exp(jax.nn.log_sigmoid(z) + log_after), 0.0)
        return jnp.einsum('bhqs,bshd->bqhd', a.astype(v.dtype), v)

    return from_blocks(lax.map(block, (to_blocks(q), block_starts(S))))


def forgetting_attention(q, k, v, log_f):
    S, d = q.shape[1], q.shape[3]
    scale = d ** -0.5
    spos = jnp.arange(S, dtype=jnp.int32)
    c = jnp.cumsum(log_f, axis=1)
    c_keys = jnp.transpose(c, (0, 2, 1))[:, :, None, :]

    def block(args):
        qb, cb, t0 = args
        tpos = t0 + jnp.arange(Q_BLOCK, dtype=jnp.int32)
        causal = (spos[None, :] <= tpos[:, None])[None, None]
        logits = (jnp.einsum('bqhd,bshd->bhqs', qb, k).astype(jnp.float32) * scale
                  + jnp.transpose(cb, (0, 2, 1))[..., None] - c_keys)
        p = jax.nn.softmax(jnp.where(causal, logits, -jnp.inf), axis=-1)
        return jnp.einsum('bhqs,bshd->bqhd', p.astype(v.dtype), v)

    return from_blocks(lax.map(block, (to_blocks(q), to_blocks(c), block_starts(S))))


def dsa_sparse_attention(q, k, v, iq, ik, iw, topk):
    B, S, H, d = q.shape
    G = k.shape[2]
    R = H // G
    scale = d ** -0.5
    idx_scale = IDX_DIM ** -0.5
    spos = jnp.arange(S, dtype=jnp.int32)

    def block(args):
        qb, iqb, iwb, t0 = args
        tpos = t0 + jnp.arange(Q_BLOCK, dtype=jnp.int32)
        causal = (spos[None, :] <= tpos[:, None])[None]
        rel = jax.nn.relu(jnp.einsum('bqhe,bse->bqhs', iqb, ik).astype(jnp.float32) * idx_scale)
        score = jnp.einsum('bqh,bqhs->bqs', iwb.astype(jnp.float32), rel)
        score = jnp.where(causal, score, -jnp.inf)
        _, sel = lax.top_k(score, topk)
        k_sel = jax.vmap(lambda kb, ib: kb[ib])(k, sel)
        v_sel = jax.vmap(lambda vb, ib: vb[ib])(v, sel)
        valid = (sel <= tpos[None, :, None])[:, :, None, None, :]
        qg = qb.reshape(B, Q_BLOCK, G, R, d)
        logits = jnp.einsum('bqgrd,bqkgd->bqgrk', qg, k_sel).astype(jnp.float32) * scale
        p = jax.nn.softmax(jnp.where(valid, logits, -jnp.inf), axis=-1)
        o = jnp.einsum('bqgrk,bqkgd->bqgrd', p.astype(v.dtype), v_sel)
        return o.reshape(B, Q_BLOCK, H, d)

    return from_blocks(lax.map(block, (to_blocks(q), to_blocks(iq), to_blocks(iw), block_starts(S))))


def hybrid_layer(x, cos_h, sin_h, cos_i, sin_i, topk,
                 norm_mix_g, w_in, fox_f_bias, fox_q_g, fox_k_g, dsa_q_g, dsa_k_g,
                 w_gate, w_branch, w_out, norm_ffn_g, w_ffn_gate, w_ffn_up, w_ffn_down):
    B, S, _ = x.shape
    h = rmsnorm(x, norm_mix_g)
    points = np.cumsum(IN_SIZES)[:-1].tolist()
    (sb_q, sb_k, sb_v, fox_q, fox_k, fox_v, fox_f,
     dsa_q, dsa_k, dsa_v, idx_q, idx_k, idx_w) = jnp.split(h @ w_in, points, axis=-1)
    heads = lambda a, n: a.reshape(B, S, n, -1)

    o_sb = stick_breaking_attention(heads(sb_q, N_HEADS_SB), heads(sb_k, N_HEADS_SB),
                                    heads(sb_v, N_HEADS_SB))

    log_f = jax.nn.log_sigmoid(fox_f.astype(jnp.float32) + fox_f_bias.astype(jnp.float32))
    o_fox = forgetting_attention(rmsnorm(heads(fox_q, N_HEADS_FOX), fox_q_g),
                                 rmsnorm(heads(fox_k, N_HEADS_FOX), fox_k_g),
                                 heads(fox_v, N_HEADS_FOX), log_f)

    qc = apply_rope(rmsnorm(heads(dsa_q, N_HEADS_DSA), dsa_q_g), cos_h, sin_h)
    kc = apply_rope(rmsnorm(heads(dsa_k, N_KV_DSA), dsa_k_g), cos_h, sin_h)
    iq = apply_rope(heads(idx_q, IDX_HEADS), cos_i, sin_i)
    ik = apply_rope(idx_k[:, :, None, :], cos_i, sin_i)[:, :, 0, :]
    iw = idx_w * (IDX_HEADS ** -0.5)
    o_dsa = dsa_sparse_attention(qc, kc, heads(dsa_v, N_KV_DSA), iq, ik, iw, topk)

    merged = None
    for i, o in enumerate((o_sb, o_fox, o_dsa)):
        y = jax.nn.sigmoid(h @ w_gate[i]) * (o.reshape(B, S, BRANCH_WIDTH) @ w_branch[i])
        merged = y if merged is None else merged + y
    x = x + merged @ w_out

    h2 = rmsnorm(x, norm_ffn_g)
    return x + (jax.nn.silu(h2 @ w_ffn_gate) * (h2 @ w_ffn_up)) @ w_ffn_down


def setup_inputs(seed: int = 0) -> dict:
    key = jax.random.key(seed)
    ks = jax.random.split(key, 16)
    f32 = jnp.float32

    def dense(k, shape, fan_in):
        return jax.random.normal(k, shape, f32) * (fan_in ** -0.5)

    def gain(k, shape):
        return 1.0 + 0.02 * jax.random.normal(k, shape, f32)

    return {
        "x": jax.random.normal(ks[0], (BATCH, SEQ, D_MODEL), f32),
        "norm_mix_g": gain(ks[1], (DEPTH, D_MODEL)),
        "w_in": dense(ks[2], (DEPTH, D_MODEL, D_IN), D_MODEL),
        "fox_f_bias": jax.random.uniform(ks[3], (DEPTH, N_HEADS_FOX), f32, 1.0, 4.0),
        "fox_q_g": gain(ks[4], (DEPTH, HEAD_DIM)),
        "fox_k_g": gain(ks[5], (DEPTH, HEAD_DIM)),
        "dsa_q_g": gain(ks[6], (DEPTH, HEAD_DIM)),
        "dsa_k_g": gain(ks[7], (DEPTH, HEAD_DIM)),
        "w_gate": dense(ks[8], (DEPTH, N_BRANCH, D_MODEL, D_MODEL), D_MODEL),
        "w_branch": dense(ks[9], (DEPTH, N_BRANCH, BRANCH_WIDTH, D_MODEL), BRANCH_WIDTH),
        "w_out": dense(ks[10], (DEPTH, D_MODEL, D_MODEL), D_MODEL),
        "norm_ffn_g": gain(ks[11], (DEPTH, D_MODEL)),
        "w_ffn_gate": dense(ks[12], (DEPTH, D_MODEL, D_FF), D_MODEL),
        "w_ffn_up": dense(ks[13], (DEPTH, D_MODEL, D_FF), D_MODEL),
        "w_ffn_down": dense(ks[14], (DEPTH, D_FF, D_MODEL), D_FF),
    }


def reference(x, norm_mix_g, w_in, fox_f_bias, fox_q_g, fox_k_g, dsa_q_g, dsa_k_g,
              w_gate, w_branch, w_out, norm_ffn_g, w_ffn_gate, w_ffn_up, w_ffn_down):
    S = x.shape[1]
    topk = min(TOPK_MAX, S // 4)
    cos_h, sin_h = rope_tables(S, HEAD_DIM)
    cos_i, sin_i = rope_tables(S, IDX_DIM)
    for l in range(DEPTH):
        x = hybrid_layer(x, cos_h, sin_h, cos_i, sin_i, topk,
                         norm_mix_g[l], w_in[l], fox_f_bias[l], fox_q_g[l], fox_k_g[l],
                         dsa_q_g[l], dsa_k_g[l], w_gate[l], w_branch[l], w_out[l],
                         norm_ffn_g[l], w_ffn_gate[l], w_ffn_up[l], w_ffn_down[l])
    return x
```

```python
import numpy as np
import concourse.bass as bass
import concourse.mybir as mybir

F32 = mybir.dt.float32
BF16 = mybir.dt.bfloat16
AF = mybir.ActivationFunctionType
ALU = mybir.AluOpType

SAME_ENG_SYNC = True


class Slot:
    __slots__ = ("id", "nw")
    _n = 0

    def __init__(self):
        Slot._n += 1
        self.id = Slot._n
        self.nw = 0


class Res:
    __slots__ = ("name", "w", "r", "slot", "prev")

    def __init__(self, name, slot=None):
        self.name = name
        self.prev = None
        if slot is not None and slot.nw > 0:
            n = slot.nw - 1
            self.prev = (("r", slot.id, n // 1800), n % 1800)
        self.w = {}
        self.r = {}
        self.slot = slot if slot is not None else Slot()


class Op:
    __slots__ = ("eng", "fn", "waits", "chan", "seq", "marked", "is_dma", "val")

    def __init__(self, eng, fn):
        self.eng = eng
        self.fn = fn
        self.waits = {}
        self.chan = None
        self.seq = None
        self.marked = False
        self.is_dma = False
        self.val = None


class Prog:
    ENGS = ("pe", "act", "dve", "pool", "sp")

    def __init__(self, nc):
        self.nc = nc
        self.ops = {e: [] for e in self.ENGS}
        self.eng_seq = {e: 0 for e in self.ENGS}
        self.chan_ops = {}
        self.final_waits = {}
        self.free_slots = []

    def res(self, name, pooled=False):
        if pooled and self.free_slots:
            return Res(name, self.free_slots.pop())
        return Res(name)

    def release(self, r):
        self.free_slots.append(r.slot)

    def _add(self, op, reads, writes, nowaw):
        for a in reads:
            for c, s in a.w.items():
                if op.waits.get(c, -1) < s:
                    op.waits[c] = s
        for a in writes:
            for c, s in a.r.items():
                if op.waits.get(c, -1) < s:
                    op.waits[c] = s
            if a.r:
                a.w = {}
                a.r = {}
            elif not nowaw:
                for c, s in a.w.items():
                    if op.waits.get(c, -1) < s:
                        op.waits[c] = s
                a.w = {}
        return op

    def op(self, eng, fn, reads=(), writes=(), nowaw=False):
        o = Op(eng, fn)
        self._add(o, reads, writes, nowaw)
        n = self.eng_seq[eng]
        o.chan = ("e", eng, n // 30000)
        o.seq = n % 30000
        self.eng_seq[eng] += 1
        self.chan_ops.setdefault(o.chan, []).append(o)
        for a in reads:
            if a.r.get(o.chan, -1) < o.seq:
                a.r[o.chan] = o.seq
        for a in writes:
            if a.w.get(o.chan, -1) < o.seq:
                a.w[o.chan] = o.seq
        self.ops[eng].append(o)
        return o

    def dma(self, eng, fn, dst, reads=(), nowaw=False):
        o = Op(eng, fn)
        o.is_dma = True
        self._add(o, reads, (dst,), nowaw)
        if dst.prev is not None:
            pc, ps_ = dst.prev
            if o.waits.get(pc, -1) < ps_:
                o.waits[pc] = ps_
            dst.prev = None
        sl = dst.slot
        o.chan = ("r", sl.id, sl.nw // 1800)
        o.seq = sl.nw % 1800
        sl.nw += 1
        self.chan_ops.setdefault(o.chan, []).append(o)
        for a in reads:
            if a.r.get(o.chan, -1) < o.seq:
                a.r[o.chan] = o.seq
        if dst.w.get(o.chan, -1) < o.seq:
            dst.w[o.chan] = o.seq
        self.ops[eng].append(o)
        return o

    def finish(self, outs, eng="sp"):
        o = Op(eng, None)
        for a in outs:
            for c, s in a.w.items():
                if o.waits.get(c, -1) < s:
                    o.waits[c] = s
        o.chan = None
        self.ops[eng].append(o)

    def emit(self):
        nc = self.nc
        for e in self.ENGS:
            seen = {}
            for o in self.ops[e]:
                keep = {}
                for c, s in o.waits.items():
                    if c[0] == "e" and c[1] == e:
                        if e == "pe" or not SAME_ENG_SYNC:
                            continue
                    if seen.get(c, -1) >= s:
                        continue
                    seen[c] = s
                    keep[c] = s
                o.waits = keep
                for c, s in keep.items():
                    self.chan_ops[c][s].marked = True
        chan_val = {}
        for c, lst in self.chan_ops.items():
            v = 0
            for o in lst:
                if c[0] == "r":
                    v += 16
                    o.val = v
                elif o.marked:
                    v += 1
                    o.val = v
                else:
                    o.val = None
            chan_val[c] = v
        used = set()
        for e in self.ENGS:
            for o in self.ops[e]:
                used.update(o.waits.keys())
        sems = {}
        for c in sorted(used, key=str):
            sems[c] = nc.alloc_semaphore(name="s_%s_%s_%s" % (c[0], c[1], c[2]))
        self.sems = sems
        self.n_sems = len(sems)
        self.max_val = max(chan_val.values()) if chan_val else 0
        block_cm = nc.Block()
        with block_cm as block:
            def body(e):
                def f(eh):
                    for o in self.ops[e]:
                        for c, s in o.waits.items():
                            eh.wait_ge(sems[c], self.chan_ops[c][s].val)
                        if o.fn is None:
                            continue
                        ins = o.fn(eh)
                        if o.chan in sems:
                            if o.is_dma:
                                ins.then_inc(sems[o.chan], 16)
                            elif o.marked:
                                ins.then_inc(sems[o.chan], 1)
                return f
            block.tensor(body("pe"))
            block.scalar(body("act"))
            block.vector(body("dve"))
            block.gpsimd(body("pool"))
            block.sync(body("sp"))

import contextlib
import numpy as np
import concourse.bass as bass
import concourse.mybir as mybir

D = 2048
HD = 128
DFF = 5632
DIN = 8792
NEG = -30000.0
EPS = 1e-6
OFF = {}
_o = 0
for _n, _s in [("sb_q", 1024), ("sb_k", 1024), ("sb_v", 1024), ("fox_q", 1024), ("fox_k", 1024), ("fox_v", 1024),
               ("fox_f", 8), ("dsa_q", 1024), ("dsa_k", 256), ("dsa_v", 256), ("idx_q", 1024), ("idx_k", 64), ("idx_w", 16)]:
    OFF[_n] = (_o, _s)
    _o += _s
assert _o == DIN


class Ring:
    _uid = 0

    def __init__(self, P, es, nc, name, shape, dt, n, psum=False):
        Ring._uid += 1
        name = "%s_u%d_" % (name, Ring._uid)
        self.items = []
        for i in range(n):
            if psum:
                t = es.enter_context(nc.psum_tensor("%s%d" % (name, i), shape, dt))
            else:
                t = es.enter_context(nc.sbuf_tensor("%s%d" % (name, i), shape, dt))
            r = P.res("%s%d" % (name, i), pooled=True)
            es.callback(P.release, r)
            self.items.append((t, r))
        self.i = 0

    def next(self):
        it = self.items[self.i % len(self.items)]
        self.i += 1
        return it


class Ctx:
    pass


def make_ctx(nc, P, S):
    c = Ctx()
    c.nc, c.P, c.S = nc, P, S
    c.SO = S // 2
    c.NT = c.SO // 512
    SO = c.SO

    def dr(name, shape, dt):
        return nc.dram_tensor(name, shape, dt, kind="Internal").ap()
    c.wb = {
        "w_in": dr("wb_in", [D, DIN], BF16), "w_gate": dr("wb_gate", [3 * D, D], BF16),
        "w_branch": dr("wb_branch", [3 * 1024, D], BF16), "w_out": dr("wb_out", [D, D], BF16),
        "w_fg": dr("wb_fg", [D, DFF], BF16), "w_fu": dr("wb_fu", [D, DFF], BF16), "w_fd": dr("wb_fd", [DFF, D], BF16),
    }
    c.r_wb = {k: P.res("wb_" + k) for k in c.wb}
    c.hT = dr("hT", [16, 128, S], BF16); c.r_hT = P.res("hT")
    c.hTo = dr("hTo", [16, 128, SO], BF16); c.r_hTo = P.res("hTo")
    c.kT = {"sb": dr("kT_sb", [8, 128, S], BF16), "fox": dr("kT_fox", [8, 128, S], BF16), "dsa": dr("kT_dsa", [2, 128, S], BF16)}
    c.ikT = dr("ikT", [128, S], BF16)
    c.v = {"sb": dr("v_sb", [S, 1024], BF16), "fox": dr("v_fox", [S, 1024], BF16), "dsa": dr("v_dsa", [S, 256], BF16)}
    c.cT = dr("cT", [8, S], F32)
    c.r_kv = P.res("kv")
    c.qT = {"sb": dr("qT_sb", [8, 128, SO], BF16), "fox": dr("qT_fox", [8, 128, SO], BF16), "dsa": dr("qT_dsa", [8, 128, SO], BF16)}
    c.iqT = dr("iqT", [8, 128, SO], BF16)
    c.iw = dr("iw", [SO, 16], F32)
    c.r_q = P.res("q")
    c.oT = dr("oT", [24, 128, SO], BF16); c.r_oT = P.res("oT")
    c.maskT = dr("maskT", [c.NT, S // 128, 128, 512], BF16); c.r_maskT = P.res("maskT")
    c.x1 = dr("x1", [SO, D], F32); c.r_x1 = P.res("x1")
    c.h2T = dr("h2T", [16, 128, SO], BF16); c.r_h2T = P.res("h2T")
    return c


def load_consts(c, es, cin):
    nc, P = c.nc, c.P

    def sb(name, shape, dt):
        return es.enter_context(nc.sbuf_tensor(name, shape, dt))
    c.ident = sb("ident", [128, 128], BF16); c.r_const = P.res("const")
    c.identf = sb("identf", [128, 128], F32)
    c.trin = sb("trin", [128, 128], BF16)
    c.onesn = sb("onesn", [128, 128], BF16)
    c.ones = sb("ones", [128, 128], BF16)
    c.onesf = sb("onesf", [128, 128], F32)
    c.cm_strict = sb("cm_strict", [128, 8, 512], BF16)
    c.cm_add = sb("cm_add", [128, 8, 512], BF16)
    c.sel = sb("sel", [128, 2], F32)
    c.epsc = sb("epsc", [128, 1], F32)
    P.op("dve", lambda e: e.memset(c.epsc[:], EPS), writes=[c.r_const])
    c.cmq_add = sb("cmq_add", [128, 4, 1024], BF16)
    c.rot = sb("rot", [128, 128], BF16)
    c.roti = sb("roti", [128, 128], BF16)
    ld = [(c.ident, "ident"), (c.identf, "identf"), (c.trin, "trin"), (c.onesn, "onesn"), (c.ones, "ones"), (c.onesf, "onesf"),
          (c.rot, "rot"), (c.roti, "roti")]
    for t, n in ld:
        eng = "pool" if t.dtype != F32 else "sp"
        P.dma(eng, lambda e, t=t, n=n: e.dma_start(out=t[:], in_=cin[n][:, :]), c.r_const, nowaw=True)
    P.dma("sp", lambda e: e.dma_start(out=c.sel[:], in_=cin["sel"][:, :]), c.r_const, nowaw=True)
    for t, n in [(c.cm_strict, "cm_strict"), (c.cm_add, "cm_add")]:
        P.dma("pool", lambda e, t=t, n=n: e.dma_start(out=t[:], in_=cin[n].rearrange("b p t -> p b t")), c.r_const, nowaw=True)
    P.dma("pool", lambda e: e.dma_start(out=c.cmq_add[:], in_=cin["cmq_add"].rearrange("q p t -> p q t")), c.r_const, nowaw=True)
    c.cin = cin


def wprep(c, w_ap, key, K, N, row0=0):
    nc, P = c.nc, c.P
    dst = c.wb[key]
    with contextlib.ExitStack() as es:
        CW = 2048
        stg = Ring(P, es, nc, "wp_f", [128, CW], F32, 3)
        stb = Ring(P, es, nc, "wp_b", [128, CW], BF16, 3)
        i = 0
        for k0 in range(0, K, 128):
            for n0 in range(0, N, CW):
                nw = min(CW, N - n0)
                (tf, rf), (tb, rb) = stg.next(), stb.next()
                P.dma("sp", lambda e, tf=tf, k0=k0, n0=n0, nw=nw: e.dma_start(out=tf[:, :nw], in_=w_ap[k0:k0 + 128, n0:n0 + nw]), rf)
                eng = ("pool", "dve", "act")[i % 3] if False else ("pool" if i % 2 == 0 else "act")
                i += 1
                if eng == "act":
                    P.op("act", lambda e, tf=tf, tb=tb, nw=nw: e.activation(out=tb[:, :nw], in_=tf[:, :nw], func=AF.Copy), reads=[rf], writes=[rb])
                else:
                    P.op(eng, lambda e, tf=tf, tb=tb, nw=nw: e.tensor_copy(out=tb[:, :nw], in_=tf[:, :nw]), reads=[rf], writes=[rb])
                P.dma("sp", lambda e, tb=tb, k0=k0, n0=n0, nw=nw: e.dma_start(out=dst[row0 + k0:row0 + k0 + 128, n0:n0 + nw], in_=tb[:, :nw]),
                      c.r_wb[key], reads=[rb], nowaw=True)


def rmsnorm_T(c, x_ap, g_ap, ntok, dstT, r_dst, tag):
    nc, P = c.nc, c.P
    with contextlib.ExitStack() as es:
        def sb(name, shape, dt):
            return es.enter_context(nc.sbuf_tensor(tag + name, shape, dt))
        gt = sb("g", [128, D], F32); r_g = P.res("g")
        P.dma("sp", lambda e: e.dma_start(out=gt[:], in_=g_ap.partition_broadcast(128)), r_g)
        xr = Ring(P, es, nc, tag + "x", [128, D], F32, 3)
        jr = Ring(P, es, nc, tag + "j", [128, D], BF16, 2)
        sr = Ring(P, es, nc, tag + "ss", [128, 2], F32, 3)
        xbr = Ring(P, es, nc, tag + "xb", [128, D], BF16, 2)
        pr = Ring(P, es, nc, tag + "pT", [128, 8, 128], BF16, 4, psum=True)
        hr = Ring(P, es, nc, tag + "hT", [128, 16, 512], BF16, 2)
        for t0 in range(0, ntok, 512):
            hT, r_h = hr.next()
            for tb in range(4):
                r0 = t0 + tb * 128
                (xt, r_x), (jt, r_j), (ss, r_s), (xb, r_xb) = xr.next(), jr.next(), sr.next(), xbr.next()
                P.dma("sp", lambda e, xt=xt, r0=r0: e.dma_start(out=xt[:], in_=x_ap[r0:r0 + 128, :]), r_x)
                P.op("act", lambda e, xt=xt, jt=jt, ss=ss: e.activation(out=jt[:], in_=xt[:], func=AF.Square, accum_out=ss[:, 0:1]), reads=[r_x], writes=[r_j, r_s])
                P.op("act", lambda e, ss=ss: e.activation(out=ss[:, 1:2], in_=ss[:, 0:1], func=AF.Ln, scale=1.0 / D, bias=c.epsc[:, 0:1]), reads=[r_s, c.r_const], writes=[r_s])
                P.op("act", lambda e, ss=ss: e.activation(out=ss[:, 1:2], in_=ss[:, 1:2], func=AF.Exp, scale=-0.5), reads=[r_s], writes=[r_s])
                P.op("dve", lambda e, xt=xt, xb=xb, ss=ss: e.scalar_tensor_tensor(out=xb[:], in0=xt[:], scalar=ss[:, 1:2], in1=gt[:], op0=ALU.mult, op1=ALU.mult), reads=[r_x, r_s, r_g], writes=[r_xb])
                for half in range(2):
                    pT, r_p = pr.next()
                    for cc in range(8):
                        ch = half * 8 + cc
                        P.op("pe", lambda e, pT=pT, cc=cc, ch=ch, xb=xb: e.transpose(out=pT[:, cc, :], in_=xb[:, ch * 128:(ch + 1) * 128], identity=c.ident[:]),
                             reads=[r_xb, c.r_const], writes=[r_p], nowaw=True)
                    eng = "act" if half == 0 else "pool_no"
                    if half == 0:
                        P.op("act", lambda e, pT=pT, hT=hT, tb=tb, half=half: e.activation(out=hT[:, half * 8:(half + 1) * 8, tb * 128:(tb + 1) * 128], in_=pT[:], func=AF.Copy), reads=[r_p], writes=[r_h], nowaw=True)
                    else:
                        P.op("dve", lambda e, pT=pT, hT=hT, tb=tb, half=half: e.tensor_copy(out=hT[:, half * 8:(half + 1) * 8, tb * 128:(tb + 1) * 128], in_=pT[:]), reads=[r_p], writes=[r_h], nowaw=True)
            P.dma("sp", lambda e, hT=hT, t0=t0: e.dma_start(out=dstT[:, :, t0:t0 + 512].rearrange("c p t -> p c t"), in_=hT[:]), r_dst, reads=[r_h], nowaw=True)


def gemm_fm(c, es_tag, actT, r_act, KC, T, w_ap, r_w, ncol0, ncols, evac, wring, pring):
    nc, P = c.nc, c.P
    for n0 in range(0, ncols, 512):
        nw = min(512, ncols - n0)
        wt, r_wt = wring.next()
        P.dma("sp", lambda e, wt=wt, n0=n0, nw=nw: e.dma_start(out=wt[:, :KC, :nw], in_=w_ap[0:KC * 128, ncol0 + n0:ncol0 + n0 + nw].rearrange("(k p) n -> p k n", p=128)), r_wt, reads=[r_w])
        for nb in range((nw + 127) // 128):
            m = min(128, nw - nb * 128)
            for ts in range(T // 512):
                ps, r_ps = pring.next()
                for kc in range(KC):
                    P.op("pe", lambda e, ps=ps, wt=wt, kc=kc, nb=nb, m=m, ts=ts: e.matmul(ps[:m, :], lhsT=wt[:, kc, nb * 128:nb * 128 + m], rhs=actT[:, kc, ts * 512:(ts + 1) * 512], start=(kc == 0), stop=(kc == KC - 1)),
                         reads=[r_wt, r_act], writes=[r_ps], nowaw=True)
                evac(ps, r_ps, (n0 // 128) + nb, ts, m)


def gemm_tm(c, actT_fn, r_act, KC, T, w_ap, r_w, ncol0, ncols, evac, wring, pring, KG=16):
    nc, P = c.nc, c.P
    NTB = T // 128
    for n0 in range(0, ncols, 512):
        nw = min(512, ncols - n0)
        pss = [pring.next() for _ in range(NTB)]
        ngr = (KC + KG - 1) // KG
        for g in range(ngr):
            kg = min(KG, KC - g * KG)
            wt, r_wt = wring.next()
            P.dma("sp", lambda e, wt=wt, n0=n0, nw=nw, g=g, kg=kg: e.dma_start(out=wt[:, :kg, :nw], in_=w_ap[g * KG * 128:(g * KG + kg) * 128, ncol0 + n0:ncol0 + n0 + nw].rearrange("(k p) n -> p k n", p=128)), r_wt, reads=[r_w])
            for tb in range(NTB):
                ps, r_ps = pss[tb]
                for k in range(kg):
                    kc = g * KG + k
                    P.op("pe", lambda e, ps=ps, wt=wt, k=k, kc=kc, tb=tb, nw=nw: e.matmul(ps[:, :nw], lhsT=actT_fn(kc, tb), rhs=wt[:, k, :nw], start=(kc == 0), stop=(kc == KC - 1)),
                         reads=[r_wt, r_act], writes=[r_ps], nowaw=True)
        for tb in range(NTB):
            evac(pss[tb][0], pss[tb][1], tb, n0, nw)


def _load_col(c, es, name, ap1d, mul):
    nc, P = c.nc, c.P
    t = es.enter_context(nc.sbuf_tensor(name, [128, 2], F32))
    r = P.res(name)
    P.dma("sp", lambda e: e.dma_start(out=t[:, 0:1], in_=ap1d.rearrange("(p o) -> p o", o=1)), r)
    P.op("dve", lambda e: e.tensor_scalar(out=t[:, 1:2], in0=t[:, 0:1], scalar1=float(mul), scalar2=None, op0=ALU.mult), reads=[r], writes=[r])
    return t, r


def proj_phase(c, lw, own):
    nc, P, S = c.nc, c.P, c.S
    NTOK = c.SO if own else S
    T = min(1024, NTOK)
    w_in, r_w = c.wb["w_in"], c.r_wb["w_in"]
    srcT, r_src = (c.hTo, c.r_hTo) if own else (c.hT, c.r_hT)
    r_dst = c.r_q if own else c.r_kv
    cin = c.cin
    with contextlib.ExitStack() as es:
        def sb(name, shape, dt):
            return es.enter_context(nc.sbuf_tensor(("pq_" if own else "pk_") + name, shape, dt))
        tg = "pq_" if own else "pk_"
        actr = Ring(P, es, nc, tg + "act", [128, 16, T], BF16, 1)
        wring = Ring(P, es, nc, tg + "w", [128, 16, 512], BF16, 3)
        pring = Ring(P, es, nc, tg + "ps", [128, 512], F32, 4, psum=True)
        paux = Ring(P, es, nc, tg + "pa", [128, 512], F32, 3, psum=True)
        stage = Ring(P, es, nc, tg + "st", [128, T], BF16, 3)
        sqr = Ring(P, es, nc, tg + "sq", [128, 512], BF16, 2)
        rr = Ring(P, es, nc, tg + "r", [128, 512], F32, 2)
        khr = Ring(P, es, nc, tg + "kh", [128, 512], BF16, 2)
        t1r = Ring(P, es, nc, tg + "t1", [128, 512], F32, 2)
        t2r = Ring(P, es, nc, tg + "t2", [128, 512], F32, 2)
        cosh = sb("cosh", [128, T], F32); sinh = sb("sinh", [128, T], F32)
        cosi = sb("cosi", [128, T], F32); sini = sb("sini", [128, T], F32)
        r_tab = P.res("tab")
        if own:
            g_fox, r_gf = _load_col(c, es, tg + "gf", lw["fox_q_g"], 128.0 ** -0.5)
            g_dsa, r_gd = _load_col(c, es, tg + "gd", lw["dsa_q_g"], 128.0 ** -0.5)
        else:
            g_fox, r_gf = _load_col(c, es, tg + "gf", lw["fox_k_g"], 1.0)
            g_dsa, r_gd = _load_col(c, es, tg + "gd", lw["dsa_k_g"], 1.0)
            fb = sb("fb", [8, 2], F32); r_fb = P.res("fb")
            P.dma("sp", lambda e: e.dma_start(out=fb[:, 0:1], in_=lw["fox_f_bias"].rearrange("(p o) -> p o", o=1)), r_fb)
            P.op("dve", lambda e: e.tensor_scalar(out=fb[:, 1:2], in0=fb[:, 0:1], scalar1=-1.0, scalar2=None, op0=ALU.mult), reads=[r_fb], writes=[r_fb])
            carry = sb("carry", [8, 1], F32); r_carry = P.res("carry")
            P.op("dve", lambda e: e.memset(carry[:], 0.0), writes=[r_carry])
            ones8 = sb("ones8", [8, 512], F32); r_ones8 = P.res("ones8")
            P.op("dve", lambda e: e.memset(ones8[:], 1.0), writes=[r_ones8])
            lfr = Ring(P, es, nc, tg + "lf", [8, 512], F32, 2)
            cTr = Ring(P, es, nc, tg + "cTs", [8, T], F32, 2)
            vst = Ring(P, es, nc, tg + "vst", [128, 512], BF16, 3)
            wik = sb("wik", [128, 16, 128], BF16); r_wik = P.res("wik")
        if own:
            wst = Ring(P, es, nc, tg + "wst", [128, 16], F32, 3)
        sfx = "_own" if own else ""
        def do_chunk(ch):
            tk0 = ch * T
            actT, r_act = actr.next()
            P.dma("sp", lambda e, actT=actT, tk0=tk0: e.dma_start(out=actT[:], in_=srcT[:, :, tk0:tk0 + T].rearrange("c p t -> p c t")), r_act, reads=[r_src])
            for tt, nm in [(cosh, "cos_h"), (sinh, "sin_h"), (cosi, "cos_i"), (sini, "sin_i")]:
                P.dma("sp", lambda e, tt=tt, nm=nm, tk0=tk0: e.dma_start(out=tt[:], in_=cin[nm + sfx][:, tk0:tk0 + T]), r_tab, nowaw=True)

            def plain_evac(dst3, scale):
                st = {}

                def ev(ps, r_ps, nb, ts, m):
                    if ts == 0:
                        st[nb] = stage.next()
                    sg, r_sg = st[nb]
                    P.op("act", lambda e: e.activation(out=sg[:, ts * 512:(ts + 1) * 512], in_=ps[:], func=AF.Copy, scale=float(scale)), reads=[r_ps], writes=[r_sg], nowaw=True)
                    if ts == T // 512 - 1:
                        P.dma("sp", lambda e: e.dma_start(out=dst3[nb, :, tk0:tk0 + T], in_=sg[:]), r_dst, reads=[r_sg], nowaw=True)
                return ev

            def norm_evac(dst3, gcol, r_gc, rope, rotm=None, cs=None):
                st = {}

                def ev(ps, r_ps, nb, ts, m):
                    if ts == 0:
                        st[nb] = stage.next()
                    sg, r_sg = st[nb]
                    tsl = slice(ts * 512, (ts + 1) * 512)
                    if gcol is not None:
                        (sq, r_sq), (pa, r_pa), (rt, r_rt) = sqr.next(), paux.next(), rr.next()
                        P.op("act", lambda e: e.activation(out=sq[:], in_=ps[:], func=AF.Square), reads=[r_ps], writes=[r_sq])
                        P.op("pe", lambda e: e.matmul(pa[:], lhsT=c.ones[:], rhs=sq[:], start=True, stop=True), reads=[r_sq, c.r_const], writes=[r_pa])
                        P.op("act", lambda e: e.activation(out=rt[:], in_=pa[:], func=AF.Ln, scale=1.0 / 128.0, bias=c.epsc[:, 0:1]), reads=[r_pa, c.r_const], writes=[r_rt])
                        P.op("act", lambda e: e.activation(out=rt[:], in_=rt[:], func=AF.Exp, scale=-0.5), reads=[r_rt], writes=[r_rt])
                        if not rope:
                            P.op("dve", lambda e: e.scalar_tensor_tensor(out=sg[:, tsl], in0=ps[:], scalar=gcol[:, 1:2], in1=rt[:], op0=ALU.mult, op1=ALU.mult), reads=[r_ps, r_rt, r_gc], writes=[r_sg], nowaw=True)
                            kh = None
                        else:
                            kh, r_kh = khr.next()
                            P.op("dve", lambda e: e.scalar_tensor_tensor(out=kh[:], in0=ps[:], scalar=gcol[:, 1:2], in1=rt[:], op0=ALU.mult, op1=ALU.mult), reads=[r_ps, r_rt, r_gc], writes=[r_kh])
                    else:
                        kh, r_kh = khr.next()
                        P.op("act", lambda e: e.activation(out=kh[:], in_=ps[:], func=AF.Copy, scale=float(cs)), reads=[r_ps], writes=[r_kh])
                    if rope:
                        ct, stb = (cosh, sinh) if rotm is c.rot else (cosi, sini)
                        (pa2, r_pa2), (t1, r_t1), (t2, r_t2) = paux.next(), t1r.next(), t2r.next()
                        P.op("pe", lambda e: e.matmul(pa2[:], lhsT=rotm[:], rhs=kh[:], start=True, stop=True), reads=[r_kh, c.r_const], writes=[r_pa2])
                        P.op("pool", lambda e: e.tensor_tensor(out=t1[:], in0=kh[:], in1=ct[:, tsl], op=ALU.mult), reads=[r_kh, r_tab], writes=[r_t1])
                        P.op("dve", lambda e: e.tensor_tensor(out=t2[:], in0=pa2[:], in1=stb[:, tsl], op=ALU.mult), reads=[r_pa2, r_tab], writes=[r_t2])
                        P.op("pool", lambda e: e.tensor_tensor(out=sg[:, tsl], in0=t1[:], in1=t2[:], op=ALU.add), reads=[r_t1, r_t2], writes=[r_sg], nowaw=True)
                    if ts == T // 512 - 1:
                        P.dma("sp", lambda e: e.dma_start(out=dst3[nb, :, tk0:tk0 + T], in_=sg[:]), r_dst, reads=[r_sg], nowaw=True)
                return ev

            if not own:
                gemm_fm(c, tg, actT, r_act, 16, T, w_in, r_w, OFF["sb_k"][0], 1024, plain_evac(c.kT["sb"], 1.0), wring, pring)
                gemm_fm(c, tg, actT, r_act, 16, T, w_in, r_w, OFF["fox_k"][0], 1024, norm_evac(c.kT["fox"], g_fox, r_gf, False), wring, pring)
                gemm_fm(c, tg, actT, r_act, 16, T, w_in, r_w, OFF["dsa_k"][0], 256, norm_evac(c.kT["dsa"], g_dsa, r_gd, True, c.rot), wring, pring)
                o_ik = OFF["idx_k"][0]
                for hh in range(2):
                    P.dma("sp", lambda e, hh=hh: e.dma_start(out=wik[:, :, hh * 64:(hh + 1) * 64], in_=w_in[:, o_ik:o_ik + 64].rearrange("(k p) n -> p k n", p=128)), r_wik, reads=[r_w], nowaw=True)
                ikdst = c.ikT.rearrange("(o p) s -> o p s", o=1)
                ev = norm_evac(ikdst, None, None, True, c.roti, 1.0)
                for ts in range(T // 512):
                    ps, r_ps = pring.next()
                    for kc in range(16):
                        P.op("pe", lambda e, ps=ps, kc=kc, ts=ts: e.matmul(ps[:], lhsT=wik[:, kc, :], rhs=actT[:, kc, ts * 512:(ts + 1) * 512], start=(kc == 0), stop=(kc == 15)), reads=[r_wik, r_act], writes=[r_ps], nowaw=True)
                    ev(ps, r_ps, 0, ts, 128)
                o_f = OFF["fox_f"][0]
                wt, r_wt = wring.next()
                P.dma("sp", lambda e, wt=wt: e.dma_start(out=wt[:, :, 0:8], in_=w_in[:, o_f:o_f + 8].rearrange("(k p) n -> p k n", p=128)), r_wt, reads=[r_w])
                cTs, r_cTs = cTr.next()
                for ts in range(T // 512):
                    ps, r_ps = pring.next()
                    for kc in range(16):
                        P.op("pe", lambda e, ps=ps, kc=kc, ts=ts, wt=wt: e.matmul(ps[:8, :], lhsT=wt[:, kc, 0:8], rhs=actT[:, kc, ts * 512:(ts + 1) * 512], start=(kc == 0), stop=(kc == 15)), reads=[r_wt, r_act], writes=[r_ps], nowaw=True)
                    (lf, r_lf) = lfr.next()
                    P.op("act", lambda e, ps=ps, lf=lf: e.activation(out=lf[:], in_=ps[:8, :], func=AF.Exp, scale=-1.0, bias=fb[:, 1:2]), reads=[r_ps, r_fb], writes=[r_lf])
                    P.op("act", lambda e, lf=lf: e.activation(out=lf[:], in_=lf[:], func=AF.Ln, bias=1.0), reads=[r_lf], writes=[r_lf])
                    P.op("dve", lambda e, lf=lf, cTs=cTs, ts=ts: e.tensor_tensor_scan(out=cTs[:, ts * 512:(ts + 1) * 512], data0=ones8[:], data1=lf[:], initial=carry[:, 0:1], op0=ALU.mult, op1=ALU.subtract), reads=[r_lf, r_ones8, r_carry], writes=[r_cTs], nowaw=True)
                    P.op("dve", lambda e, cTs=cTs, ts=ts: e.tensor_copy(out=carry[:], in_=cTs[:, ts * 512 + 511:ts * 512 + 512]), reads=[r_cTs], writes=[r_carry])
                P.dma("sp", lambda e, cTs=cTs: e.dma_start(out=c.cT[:, tk0:tk0 + T], in_=cTs[:]), r_dst, reads=[r_cTs], nowaw=True)
                for key, ncols in [("sb", 1024), ("fox", 1024), ("dsa", 256)]:
                    vd = c.v[key]

                    def vev(ps, r_ps, tb, n0, nw, vd=vd):
                        sg, r_sg = vst.next()
                        P.op("act" if tb % 2 == 0 else "dve", (lambda e: e.activation(out=sg[:, :nw], in_=ps[:, :nw], func=AF.Copy)) if tb % 2 == 0 else (lambda e: e.tensor_copy(out=sg[:, :nw], in_=ps[:, :nw])), reads=[r_ps], writes=[r_sg])
                        P.dma("sp", lambda e: e.dma_start(out=vd[tk0 + tb * 128:tk0 + (tb + 1) * 128, n0:n0 + nw], in_=sg[:, :nw]), r_dst, reads=[r_sg], nowaw=True)
                    for tsub in range(T // 512):
                        def afn(kc, tb, tsub=tsub):
                            return actT[:, kc, tsub * 512 + tb * 128: tsub * 512 + (tb + 1) * 128]

                        def vev2(ps, r_ps, tb, n0, nw, tsub=tsub):
                            vev(ps, r_ps, tsub * 4 + tb, n0, nw)
                        gemm_tm(c, afn, r_act, 16, 512, w_in, r_w, OFF[key + "_v"][0], ncols, vev2, wring, pring)
            else:
                gemm_fm(c, tg, actT, r_act, 16, T, w_in, r_w, OFF["sb_q"][0], 1024, plain_evac(c.qT["sb"], 128.0 ** -0.5), wring, pring)
                gemm_fm(c, tg, actT, r_act, 16, T, w_in, r_w, OFF["fox_q"][0], 1024, norm_evac(c.qT["fox"], g_fox, r_gf, False), wring, pring)
                gemm_fm(c, tg, actT, r_act, 16, T, w_in, r_w, OFF["dsa_q"][0], 1024, norm_evac(c.qT["dsa"], g_dsa, r_gd, True, c.rot), wring, pring)
                gemm_fm(c, tg, actT, r_act, 16, T, w_in, r_w, OFF["idx_q"][0], 1024, norm_evac(c.iqT, None, None, True, c.roti, 0.125), wring, pring)
                for tsub in range(T // 512):
                    def afn(kc, tb, tsub=tsub):
                        return actT[:, kc, tsub * 512 + tb * 128: tsub * 512 + (tb + 1) * 128]

                    def wev(ps, r_ps, tb, n0, nw, tsub=tsub):
                        sg, r_sg = wst.next()
                        P.op("act", lambda e: e.activation(out=sg[:, :16], in_=ps[:, :16], func=AF.Copy, scale=0.25), reads=[r_ps], writes=[r_sg])
                        r0 = tk0 + tsub * 512 + tb * 128
                        P.dma("sp", lambda e: e.dma_start(out=c.iw[r0:r0 + 128, :], in_=sg[:, :16]), r_dst, reads=[r_sg], nowaw=True)
                    gemm_tm(c, afn, r_act, 16, 512, w_in, r_w, OFF["idx_w"][0], 16, wev, wring, pring)
        for ch in range(NTOK // T):
            do_chunk(ch)


NITER = 26


def attn_phase(c, mixer):
    nc, P, S, SO, NT = c.nc, c.P, c.S, c.SO, c.NT
    NB = S // 128
    mi = {"sb": 0, "fox": 1, "dsa": 2}[mixer]
    with contextlib.ExitStack() as es:
        tg = "a%d_" % mi

        def sb(name, shape, dt):
            return es.enter_context(nc.sbuf_tensor(tg + name, shape, dt))
        kring = Ring(P, es, nc, tg + "k", [128, S], BF16, 2)
        vring = Ring(P, es, nc, tg + "v", [128, NB, 128], BF16, 2)
        qring = Ring(P, es, nc, tg + "q", [128, SO], BF16, 2)
        zring = Ring(P, es, nc, tg + "z", [128, 512], F32, 2, psum=True)
        oring = Ring(P, es, nc, tg + "o", [128, 512], F32, 2, psum=True)
        dring = Ring(P, es, nc, tg + "d", [128, 512], F32, 2, psum=True)
        ering = Ring(P, es, nc, tg + "e", [128, 512], F32, 3)
        lring = Ring(P, es, nc, tg + "l", [128, 512], BF16, 3)
        xring = Ring(P, es, nc, tg + "x", [128, 512], F32, 3)
        pring = Ring(P, es, nc, tg + "p", [128, 512], BF16, 3)
        ostage = Ring(P, es, nc, tg + "os", [128, 512], BF16, 2)
        if mixer == "sb":
            Rt = sb("R", [128, 512], F32); r_R = P.res("R")
        if mixer == "fox":
            ccol = sb("ccol", [128, NB, 8], F32); r_ccol = P.res("ccol")
            with contextlib.ExitStack() as es2:
                cTs = es2.enter_context(nc.sbuf_tensor(tg + "cTs", [8, S], F32)); r_cTs = P.res("cTs")
                pc = es2.enter_context(nc.psum_tensor(tg + "pc", [128, NB, 8], F32)); r_pc = P.res("pc")
                for s0 in range(0, S, 2048):
                    s1 = min(S, s0 + 2048)
                    P.dma("sp", lambda e, s0=s0, s1=s1: e.dma_start(out=cTs[:, s0:s1], in_=c.cT[:, s0:s1]), r_cTs, reads=[c.r_kv], nowaw=True)
                for b in range(NB):
                    P.op("pe", lambda e, b=b: e.transpose(out=pc[:, b, :], in_=cTs[0:8, b * 128:(b + 1) * 128], identity=c.identf[0:8, 0:8]), reads=[r_cTs, c.r_const], writes=[r_pc], nowaw=True)
                P.op("dve", lambda e: e.tensor_copy(out=ccol[:], in_=pc[:]), reads=[r_pc], writes=[r_ccol])
            ctbr = Ring(P, es, nc, tg + "ctb", [128, 512], F32, 2)
            ctar = Ring(P, es, nc, tg + "cta", [128, 2, 512], F32, 2)
            crefr = Ring(P, es, nc, tg + "cref", [128, 1], F32, 2)
            biasr = Ring(P, es, nc, tg + "bias", [128, NB], F32, 2)
            rdr = Ring(P, es, nc, tg + "rd", [128, 512], F32, 2)
        if mixer == "dsa":
            mring = Ring(P, es, nc, tg + "m", [128, 8, 512], BF16, 2)
            rdr = Ring(P, es, nc, tg + "rd", [128, 512], F32, 2)
        for h in range(8):
            g = (h // 4) if mixer == "dsa" else h
            (kT, r_k), (vt, r_v), (qT, r_qT) = kring.next(), vring.next(), qring.next()
            for s0 in range(0, S, 2048):
                s1 = min(S, s0 + 2048)
                P.dma("sp", lambda e, kT=kT, g=g, s0=s0, s1=s1: e.dma_start(out=kT[:, s0:s1], in_=c.kT[mixer][g, :, s0:s1]), r_k, reads=[c.r_kv], nowaw=True)
            for b0 in range(0, NB, 8):
                b1 = min(NB, b0 + 8)
                P.dma("sp", lambda e, vt=vt, g=g, b0=b0, b1=b1: e.dma_start(out=vt[:, b0:b1, :], in_=c.v[mixer][b0 * 128:b1 * 128, g * 128:(g + 1) * 128].rearrange("(b p) d -> p b d", p=128)), r_v, reads=[c.r_kv], nowaw=True)
            for s0 in range(0, SO, 2048):
                s1 = min(SO, s0 + 2048)
                P.dma("sp", lambda e, qT=qT, h=h, s0=s0, s1=s1: e.dma_start(out=qT[:, s0:s1], in_=c.qT[mixer][h, :, s0:s1]), r_qT, reads=[c.r_q], nowaw=True)
            for m in range(NT):
                KB = 8 * (m + 1)
                qs = slice(m * 512, (m + 1) * 512)
                o_ps, r_o = oring.next()
                if mixer == "sb":
                    order = list(reversed(range(KB)))
                    P.op("pool", lambda e: e.memset(Rt[:], 0.0), writes=[r_R])
                else:
                    order = list(range(KB))
                    d_ps, r_d = dring.next()
                if mixer == "fox":
                    (cta, r_cta), (ctb, r_ctb), (cref, r_cref), (bias, r_bias) = ctar.next(), ctbr.next(), crefr.next(), biasr.next()
                    for pp in range(2):
                        g0 = 1024 * m + 512 * pp
                        P.dma("sp", lambda e, cta=cta, pp=pp, g0=g0, h=h: e.dma_start(out=cta[:, pp, :], in_=c.cT[h, g0:g0 + 512].partition_broadcast(128)), r_cta, reads=[c.r_kv], nowaw=True)
                    P.op("dve", lambda e, cta=cta, ctb=ctb: e.tensor_scalar(out=ctb[:], in0=cta[:, 0, :], scalar1=c.sel[:, 0:1], scalar2=None, op0=ALU.mult), reads=[r_cta, c.r_const], writes=[r_ctb])
                    P.op("dve", lambda e, cta=cta, ctb=ctb: e.scalar_tensor_tensor(out=ctb[:], in0=cta[:, 1, :], scalar=c.sel[:, 1:2], in1=ctb[:], op0=ALU.mult, op1=ALU.add), reads=[r_cta, c.r_const, r_ctb], writes=[r_ctb])
                    P.op("dve", lambda e, cref=cref, ctb=ctb: e.tensor_copy(out=cref[:], in_=ctb[:, 0:1]), reads=[r_ctb], writes=[r_cref])
                    P.op("dve", lambda e, cref=cref, ctb=ctb: e.tensor_scalar(out=ctb[:], in0=ctb[:], scalar1=cref[:, 0:1], scalar2=None, op0=ALU.subtract), reads=[r_ctb, r_cref], writes=[r_ctb])
                    P.op("dve", lambda e, cref=cref, bias=bias, KB=KB, h=h: e.tensor_scalar(out=bias[:, :KB], in0=ccol[:, :KB, h], scalar1=-1.0, scalar2=cref[:, 0:1], op0=ALU.mult, op1=ALU.add), reads=[r_ccol, r_cref], writes=[r_bias])
                for i, kb in enumerate(order):
                    dg = kb - (KB - 8)
                    ks = slice(kb * 128, (kb + 1) * 128)
                    first, last = (i == 0), (i == len(order) - 1)
                    z_ps, r_z = zring.next()
                    if mixer == "sb":
                        (et, r_e), (Lt, r_L), (Xt, r_X), (Pm, r_P) = ering.next(), lring.next(), xring.next(), pring.next()
                        s_ps, r_s = dring.next()
                        P.op("pe", lambda e, z_ps=z_ps, kT=kT, qT=qT, ks=ks, qs=qs: e.matmul(z_ps[:], lhsT=kT[:, ks], rhs=qT[:, qs], start=True, stop=False), reads=[r_k, r_qT], writes=[r_z])
                        P.op("act", lambda e, z_ps=z_ps, et=et: e.activation(out=et[:], in_=z_ps[:], func=AF.Exp), reads=[r_z], writes=[r_e])
                        P.op("act", lambda e, et=et, Lt=Lt: e.activation(out=Lt[:], in_=et[:], func=AF.Ln, bias=1.0), reads=[r_e], writes=[r_L])
                        if dg >= 0:
                            P.op("pool", lambda e, Lt=Lt, dg=dg: e.tensor_tensor(out=Lt[:], in0=Lt[:], in1=c.cm_strict[:, dg, :], op=ALU.mult), reads=[r_L, c.r_const], writes=[r_L])
                        P.op("pe", lambda e, z_ps=z_ps, Lt=Lt: e.matmul(z_ps[:], lhsT=c.trin[:], rhs=Lt[:], start=False, stop=True), reads=[r_L, c.r_const], writes=[r_z])
                        if not last:
                            P.op("pe", lambda e, s_ps=s_ps, Lt=Lt: e.matmul(s_ps[:], lhsT=c.onesn[:], rhs=Lt[:], start=True, stop=True), reads=[r_L, c.r_const], writes=[r_s])
                        P.op("dve", lambda e, z_ps=z_ps, Xt=Xt: e.tensor_tensor(out=Xt[:], in0=z_ps[:], in1=Rt[:], op=ALU.add), reads=[r_z, r_R], writes=[r_X])
                        if not last:
                            P.op("dve", lambda e, s_ps=s_ps: e.tensor_tensor(out=Rt[:], in0=s_ps[:], in1=Rt[:], op=ALU.add), reads=[r_s, r_R], writes=[r_R])
                        P.op("act", lambda e, Xt=Xt, Pm=Pm: e.activation(out=Pm[:], in_=Xt[:], func=AF.Exp), reads=[r_X], writes=[r_P])
                        if dg >= 0:
                            P.op("pool", lambda e, Pm=Pm, dg=dg: e.tensor_tensor(out=Pm[:], in0=Pm[:], in1=c.cm_strict[:, dg, :], op=ALU.mult), reads=[r_P, c.r_const], writes=[r_P])
                        P.op("pe", lambda e, o_ps=o_ps, vt=vt, Pm=Pm, kb=kb, first=first, last=last: e.matmul(o_ps[:], lhsT=vt[:, kb, :], rhs=Pm[:], start=first, stop=last), reads=[r_v, r_P], writes=[r_o], nowaw=True)
                    elif mixer == "fox":
                        (Xt, r_X), (Pm, r_P) = xring.next(), pring.next()
                        P.op("pe", lambda e, z_ps=z_ps, kT=kT, qT=qT, ks=ks, qs=qs: e.matmul(z_ps[:], lhsT=kT[:, ks], rhs=qT[:, qs], start=True, stop=True), reads=[r_k, r_qT], writes=[r_z])
                        P.op("dve", lambda e, z_ps=z_ps, Xt=Xt, ctb=ctb: e.tensor_tensor(out=Xt[:], in0=z_ps[:], in1=ctb[:], op=ALU.add), reads=[r_z, r_ctb], writes=[r_X])
                        if dg >= 0:
                            P.op("pool", lambda e, Xt=Xt, dg=dg: e.tensor_tensor(out=Xt[:], in0=Xt[:], in1=c.cm_add[:, dg, :], op=ALU.add), reads=[r_X, c.r_const], writes=[r_X])
                        P.op("act", lambda e, Xt=Xt, Pm=Pm, bias=bias, kb=kb: e.activation(out=Pm[:], in_=Xt[:], func=AF.Exp, bias=bias[:, kb:kb + 1]), reads=[r_X, r_bias], writes=[r_P])
                    else:
                        (Et, r_E), (Pm, r_P) = lring.next(), pring.next()
                        if kb % 8 == 0:
                            mt, r_mt = mring.next()
                            P.dma("sp", lambda e, mt=mt, m=m, kb=kb: e.dma_start(out=mt[:], in_=c.maskT[m, kb:kb + 8].rearrange("b p t -> p b t")), r_mt, reads=[c.r_maskT])
                        P.op("pe", lambda e, z_ps=z_ps, kT=kT, qT=qT, ks=ks, qs=qs: e.matmul(z_ps[:], lhsT=kT[:, ks], rhs=qT[:, qs], start=True, stop=True), reads=[r_k, r_qT], writes=[r_z])
                        P.op("act", lambda e, z_ps=z_ps, Et=Et: e.activation(out=Et[:], in_=z_ps[:], func=AF.Exp), reads=[r_z], writes=[r_E])
                        P.op("pool", lambda e, Et=Et, Pm=Pm, mt=mt, kb=kb: e.tensor_tensor(out=Pm[:], in0=Et[:], in1=mt[:, kb % 8, :], op=ALU.mult), reads=[r_E, r_mt], writes=[r_P])
                    if mixer != "sb":
                        P.op("pe", lambda e, o_ps=o_ps, vt=vt, Pm=Pm, kb=kb, first=first, last=last: e.matmul(o_ps[:], lhsT=vt[:, kb, :], rhs=Pm[:], start=first, stop=last), reads=[r_v, r_P], writes=[r_o], nowaw=True)
                        P.op("pe", lambda e, d_ps=d_ps, Pm=Pm, first=first, last=last: e.matmul(d_ps[:], lhsT=c.ones[:], rhs=Pm[:], start=first, stop=last), reads=[r_P, c.r_const], writes=[r_d], nowaw=True)
                os_, r_os = ostage.next()
                if mixer == "sb":
                    P.op("act", lambda e, o_ps=o_ps, os_=os_: e.activation(out=os_[:], in_=o_ps[:], func=AF.Copy), reads=[r_o], writes=[r_os])
                else:
                    rd, r_rd = rdr.next()
                    P.op("dve", lambda e, d_ps=d_ps, rd=rd: e.reciprocal(out=rd[:], in_=d_ps[:]), reads=[r_d], writes=[r_rd])
                    P.op("dve", lambda e, o_ps=o_ps, rd=rd, os_=os_: e.tensor_tensor(out=os_[:], in0=o_ps[:], in1=rd[:], op=ALU.mult), reads=[r_o, r_rd], writes=[r_os])
                P.dma("sp", lambda e, os_=os_, h=h, qs=qs: e.dma_start(out=c.oT[mi * 8 + h, :, qs], in_=os_[:]), c.r_oT, reads=[r_os], nowaw=True)


def indexer_phase(c):
    nc, P, S, SO, NT = c.nc, c.P, c.S, c.SO, c.NT
    with contextlib.ExitStack() as es:
        tg = "ix_"

        def sb(name, shape, dt):
            return es.enter_context(nc.sbuf_tensor(tg + name, shape, dt))
        ikT = sb("ik", [128, S], BF16); r_ik = P.res("ik")
        for s0 in range(0, S, 2048):
            s1 = min(S, s0 + 2048)
            P.dma("sp", lambda e, s0=s0, s1=s1: e.dma_start(out=ikT[:, s0:s1], in_=c.ikT[:, s0:s1]), r_ik, reads=[c.r_kv], nowaw=True)
        score = sb("score", [128, S], F32); r_sc = P.res("score")
        mk = sb("mk", [128, S], BF16); r_mk = P.res("mk")
        mstage = sb("mstage", [128, S // 128, 512], BF16); r_ms = P.res("mstage")
        iqr = Ring(P, es, nc, tg + "iq", [128, 8, 512], BF16, 2)
        iwr = Ring(P, es, nc, tg + "iw", [128, 4, 16], F32, 2)
        dgr = Ring(P, es, nc, tg + "dg", [128, 16, 128], BF16, 2)
        rlr = Ring(P, es, nc, tg + "rl", [128, 512], BF16, 4)
        apr = Ring(P, es, nc, tg + "ap", [128, 512], F32, 3, psum=True)
        accr = Ring(P, es, nc, tg + "acc", [128, 512], F32, 2, psum=True)
        ptr = Ring(P, es, nc, tg + "pt", [128, 8, 128], BF16, 2, psum=True)
        sm = sb("sm", [128, 16], F32); r_sm = P.res("sm")
        for m in range(NT):
            KB = 8 * (m + 1)
            Lk = 128 * KB
            NCk = Lk // 512
            (iq, r_iq), (iwt, r_iw) = iqr.next(), iwr.next()
            P.dma("sp", lambda e, iq=iq, m=m: e.dma_start(out=iq[:], in_=c.iqT[:, :, m * 512:(m + 1) * 512].rearrange("j p t -> p j t")), r_iq, reads=[c.r_q])
            P.dma("sp", lambda e, iwt=iwt, m=m: e.dma_start(out=iwt[:], in_=c.iw[m * 512:(m + 1) * 512, :].rearrange("(q p) h -> p q h", p=128)), r_iw, reads=[c.r_q])
            for qb in range(4):
                dg, r_dg = dgr.next()
                for h in range(16):
                    P.op("pool", lambda e, dg=dg, h=h, iwt=iwt, qb=qb: e.tensor_scalar(out=dg[:, h, :], in0=c.ident[:], scalar1=iwt[:, qb, h:h + 1], scalar2=None, op0=ALU.mult), reads=[r_iw, c.r_const], writes=[r_dg], nowaw=True)
                for kc in range(NCk):
                    acc, r_acc = accr.next()
                    for h in range(16):
                        j, hb = h // 2, (h % 2) * 64
                        (ap_, r_ap), (rl, r_rl) = apr.next(), rlr.next()
                        P.op("pe", lambda e, ap_=ap_, iq=iq, j=j, hb=hb, qb=qb, kc=kc: e.matmul(ap_[:], lhsT=iq[hb:hb + 64, j, qb * 128:(qb + 1) * 128], rhs=ikT[hb:hb + 64, kc * 512:(kc + 1) * 512], start=True, stop=True), reads=[r_iq, r_ik], writes=[r_ap])
                        P.op("act", lambda e, ap_=ap_, rl=rl: e.activation(out=rl[:], in_=ap_[:], func=AF.Relu), reads=[r_ap], writes=[r_rl])
                        P.op("pe", lambda e, acc=acc, dg=dg, h=h, rl=rl: e.matmul(acc[:], lhsT=dg[:, h, :], rhs=rl[:], start=(h == 0), stop=(h == 15)), reads=[r_dg, r_rl], writes=[r_acc], nowaw=True)
                    ksl = slice(kc * 512, (kc + 1) * 512)
                    if kc >= NCk - 2:
                        dd = kc - (NCk - 2)
                        P.op("dve", lambda e, acc=acc, ksl=ksl, qb=qb, dd=dd: e.tensor_tensor(out=score[:, ksl], in0=acc[:], in1=c.cmq_add[:, qb, dd * 512:(dd + 1) * 512], op=ALU.add), reads=[r_acc, c.r_const], writes=[r_sc], nowaw=True)
                    else:
                        P.op("dve", lambda e, acc=acc, ksl=ksl: e.tensor_copy(out=score[:, ksl], in_=acc[:]), reads=[r_acc], writes=[r_sc], nowaw=True)
                P.op("dve", lambda e, Lk=Lk: e.max(out=sm[:, 0:8], in_=score[:, :Lk]), reads=[r_sc], writes=[r_sm])
                P.op("dve", lambda e: e.memset(sm[:, 8:9], -64.0), writes=[r_sm])
                P.op("dve", lambda e: e.tensor_scalar(out=sm[:, 9:10], in0=sm[:, 0:1], scalar1=64.0, scalar2=None, op0=ALU.add), reads=[r_sm], writes=[r_sm])
                for it in range(NITER):
                    f = 0.5 ** (it + 1)
                    P.op("dve", lambda e, f=f: e.scalar_tensor_tensor(out=sm[:, 10:11], in0=sm[:, 9:10], scalar=f, in1=sm[:, 8:9], op0=ALU.mult, op1=ALU.add), reads=[r_sm], writes=[r_sm])
                    P.op("dve", lambda e, Lk=Lk: e.tensor_scalar(out=mk[:, :Lk], in0=score[:, :Lk], scalar1=sm[:, 10:11], scalar2=0.0, op0=ALU.is_ge, op1=ALU.add, accum_out=sm[:, 11:12]), reads=[r_sc, r_sm], writes=[r_mk, r_sm])
                    P.op("dve", lambda e, f=f: e.tensor_scalar(out=sm[:, 12:13], in0=sm[:, 11:12], scalar1=255.5, scalar2=f, op0=ALU.is_ge, op1=ALU.mult), reads=[r_sm], writes=[r_sm])
                    P.op("dve", lambda e: e.scalar_tensor_tensor(out=sm[:, 8:9], in0=sm[:, 9:10], scalar=sm[:, 12:13], in1=sm[:, 8:9], op0=ALU.mult, op1=ALU.add), reads=[r_sm], writes=[r_sm])
                P.op("dve", lambda e, Lk=Lk: e.tensor_scalar(out=mk[:, :Lk], in0=score[:, :Lk], scalar1=sm[:, 8:9], scalar2=None, op0=ALU.is_ge), reads=[r_sc, r_sm], writes=[r_mk])
                for k0 in range(0, KB, 8):
                    pt, r_pt = ptr.next()
                    for k in range(8):
                        kb = k0 + k
                        P.op("pe", lambda e, pt=pt, k=k, kb=kb: e.transpose(out=pt[:, k, :], in_=mk[:, kb * 128:(kb + 1) * 128], identity=c.ident[:]), reads=[r_mk, c.r_const], writes=[r_pt], nowaw=True)
                    if (k0 // 8) % 2 == 0:
                        P.op("act", lambda e, pt=pt, k0=k0, qb=qb: e.activation(out=mstage[:, k0:k0 + 8, qb * 128:(qb + 1) * 128], in_=pt[:], func=AF.Copy), reads=[r_pt], writes=[r_ms], nowaw=True)
                    else:
                        P.op("pool" if False else "dve", lambda e, pt=pt, k0=k0, qb=qb: e.tensor_copy(out=mstage[:, k0:k0 + 8, qb * 128:(qb + 1) * 128], in_=pt[:]), reads=[r_pt], writes=[r_ms], nowaw=True)
            for b0 in range(0, KB, 8):
                P.dma("sp", lambda e, m=m, b0=b0: e.dma_start(out=c.maskT[m, b0:b0 + 8].rearrange("b p t -> p b t"), in_=mstage[:, b0:b0 + 8, :]), c.r_maskT, reads=[r_ms], nowaw=True)


def merge_phase(c, x_own):
    nc, P, SO = c.nc, c.P, c.SO
    T = 512
    with contextlib.ExitStack() as es:
        tg = "mg_"
        hr = Ring(P, es, nc, tg + "h", [128, 16, T], BF16, 1)
        orr = Ring(P, es, nc, tg + "o", [128, 24, T], BF16, 1)
        wring = Ring(P, es, nc, tg + "w", [128, 16, 512], BF16, 3)
        pring = Ring(P, es, nc, tg + "ps", [128, 512], F32, 8, psum=True)
        sgb = es.enter_context(nc.sbuf_tensor(tg + "sg", [128, 8, T], BF16)); r_sgb = P.res("sgb")
        acc = es.enter_context(nc.sbuf_tensor(tg + "acc", [128, 8, T], F32)); r_acc = P.res("acc")
        mT = es.enter_context(nc.sbuf_tensor(tg + "mT", [128, 16, T], BF16)); r_mT = P.res("mT")
        tmpr = Ring(P, es, nc, tg + "tmp", [128, T], F32, 3)
        xr = Ring(P, es, nc, tg + "x", [128, D], F32, 4)
        for ch in range(SO // T):
            t0 = ch * T
            (hT, r_h), (oT, r_o) = hr.next(), orr.next()
            P.dma("sp", lambda e, hT=hT, t0=t0: e.dma_start(out=hT[:], in_=c.hTo[:, :, t0:t0 + T].rearrange("c p t -> p c t")), r_h, reads=[c.r_hTo])
            P.dma("sp", lambda e, oT=oT, t0=t0: e.dma_start(out=oT[:], in_=c.oT[:, :, t0:t0 + T].rearrange("c p t -> p c t")), r_o, reads=[c.r_oT])
            for half in range(2):
                for i in range(3):
                    def gev(ps, r_ps, nb, ts, m):
                        P.op("act", lambda e: e.activation(out=sgb[:, nb, :], in_=ps[:], func=AF.Sigmoid), reads=[r_ps], writes=[r_sgb], nowaw=True)
                    gemm_fm(c, tg, hT, r_h, 16, T, c.wb["w_gate"][i * D:(i + 1) * D, :], c.r_wb["w_gate"], half * 1024, 1024, gev, wring, pring)

                    def bev(ps, r_ps, nb, ts, m, i=i, half=half):
                        if i == 0:
                            P.op("dve", lambda e: e.tensor_tensor(out=acc[:, nb, :], in0=ps[:], in1=sgb[:, nb, :], op=ALU.mult), reads=[r_ps, r_sgb], writes=[r_acc], nowaw=True)
                        else:
                            tmp, r_tmp = tmpr.next()
                            P.op("dve", lambda e: e.tensor_tensor(out=tmp[:], in0=ps[:], in1=sgb[:, nb, :], op=ALU.mult), reads=[r_ps, r_sgb], writes=[r_tmp])
                            if i == 1:
                                P.op("pool", lambda e: e.tensor_tensor(out=acc[:, nb, :], in0=acc[:, nb, :], in1=tmp[:], op=ALU.add), reads=[r_tmp, r_acc], writes=[r_acc], nowaw=True)
                            else:
                                P.op("pool", lambda e: e.tensor_tensor(out=mT[:, half * 8 + nb, :], in0=acc[:, nb, :], in1=tmp[:], op=ALU.add), reads=[r_tmp, r_acc], writes=[r_mT], nowaw=True)
                    oTi = oT[:, i * 8:(i + 1) * 8, :]
                    gemm_fm(c, tg, oTi, r_o, 8, T, c.wb["w_branch"][i * 1024:(i + 1) * 1024, :], c.r_wb["w_branch"], half * 1024, 1024, bev, wring, pring)
            xts = []
            for tb in range(4):
                xt, r_x = xr.next()
                P.dma("sp", lambda e, xt=xt, tb=tb, t0=t0: e.dma_start(out=xt[:], in_=x_own[t0 + tb * 128:t0 + (tb + 1) * 128, :]), r_x)
                xts.append((xt, r_x))

            def afn(kc, tb):
                return mT[:, kc, tb * 128:(tb + 1) * 128]

            def oev(ps, r_ps, tb, n0, nw):
                xt, r_x = xts[tb]
                P.op("dve", lambda e: e.tensor_tensor(out=xt[:, n0:n0 + nw], in0=ps[:, :nw], in1=xt[:, n0:n0 + nw], op=ALU.add), reads=[r_ps, r_x], writes=[r_x])
            gemm_tm(c, afn, r_mT, 16, T, c.wb["w_out"], c.r_wb["w_out"], 0, D, oev, wring, pring)
            for tb in range(4):
                xt, r_x = xts[tb]
                P.dma("sp", lambda e, xt=xt, tb=tb, t0=t0: e.dma_start(out=c.x1[t0 + tb * 128:t0 + (tb + 1) * 128, :], in_=xt[:]), c.r_x1, reads=[r_x], nowaw=True)


def ffn_phase(c, y_out, r_y):
    nc, P, SO = c.nc, c.P, c.SO
    T = 512
    with contextlib.ExitStack() as es:
        tg = "ff_"
        hr = Ring(P, es, nc, tg + "h", [128, 16, T], BF16, 1)
        uT = es.enter_context(nc.sbuf_tensor(tg + "uT", [128, 44, T], BF16)); r_uT = P.res("uT")
        wring = Ring(P, es, nc, tg + "w", [128, 16, 512], BF16, 3)
        pring = Ring(P, es, nc, tg + "ps", [128, 512], F32, 8, psum=True)
        sgr = Ring(P, es, nc, tg + "sg", [128, T], F32, 3)
        xr = Ring(P, es, nc, tg + "x", [128, D], F32, 4)
        for ch in range(SO // T):
            t0 = ch * T
            hT, r_h = hr.next()
            P.dma("sp", lambda e, hT=hT, t0=t0: e.dma_start(out=hT[:], in_=c.h2T[:, :, t0:t0 + T].rearrange("c p t -> p c t")), r_h, reads=[c.r_h2T])
            for n0 in range(0, DFF, 512):
                (wg, r_wg), (wu, r_wu) = wring.next(), wring.next()
                P.dma("sp", lambda e, wg=wg, n0=n0: e.dma_start(out=wg[:], in_=c.wb["w_fg"][:, n0:n0 + 512].rearrange("(k p) n -> p k n", p=128)), r_wg, reads=[c.r_wb["w_fg"]])
                P.dma("sp", lambda e, wu=wu, n0=n0: e.dma_start(out=wu[:], in_=c.wb["w_fu"][:, n0:n0 + 512].rearrange("(k p) n -> p k n", p=128)), r_wu, reads=[c.r_wb["w_fu"]])
                for nb in range(4):
                    fb = n0 // 128 + nb
                    (gp, r_gp), (up, r_up), (sg, r_sg) = pring.next(), pring.next(), sgr.next()
                    for kc in range(16):
                        P.op("pe", lambda e, gp=gp, wg=wg, kc=kc, nb=nb, hT=hT: e.matmul(gp[:], lhsT=wg[:, kc, nb * 128:(nb + 1) * 128], rhs=hT[:, kc, :], start=(kc == 0), stop=(kc == 15)), reads=[r_wg, r_h], writes=[r_gp], nowaw=True)
                    for kc in range(16):
                        P.op("pe", lambda e, up=up, wu=wu, kc=kc, nb=nb, hT=hT: e.matmul(up[:], lhsT=wu[:, kc, nb * 128:(nb + 1) * 128], rhs=hT[:, kc, :], start=(kc == 0), stop=(kc == 15)), reads=[r_wu, r_h], writes=[r_up], nowaw=True)
                    P.op("act", lambda e, gp=gp, sg=sg: e.activation(out=sg[:], in_=gp[:], func=AF.Silu), reads=[r_gp], writes=[r_sg])
                    P.op("dve", lambda e, up=up, sg=sg, fb=fb: e.tensor_tensor(out=uT[:, fb, :], in0=up[:], in1=sg[:], op=ALU.mult), reads=[r_up, r_sg], writes=[r_uT], nowaw=True)
            xts = []
            for tb in range(4):
                xt, r_x = xr.next()
                P.dma("sp", lambda e, xt=xt, tb=tb, t0=t0: e.dma_start(out=xt[:], in_=c.x1[t0 + tb * 128:t0 + (tb + 1) * 128, :]), r_x, reads=[c.r_x1])
                xts.append((xt, r_x))

            def afn(kc, tb):
                return uT[:, kc, tb * 128:(tb + 1) * 128]

            def dev(ps, r_ps, tb, n0, nw):
                xt, r_x = xts[tb]
                P.op("dve", lambda e: e.tensor_tensor(out=xt[:, n0:n0 + nw], in0=ps[:, :nw], in1=xt[:, n0:n0 + nw], op=ALU.add), reads=[r_ps, r_x], writes=[r_x])
            gemm_tm(c, afn, r_uT, 44, T, c.wb["w_fd"], c.r_wb["w_fd"], 0, D, dev, wring, pring, KG=16)
            for tb in range(4):
                xt, r_x = xts[tb]
                P.dma("sp", lambda e, xt=xt, tb=tb, t0=t0: e.dma_start(out=y_out[t0 + tb * 128:t0 + (tb + 1) * 128, :], in_=xt[:]), r_y, reads=[r_x], nowaw=True)


def build_layer(c, lw, x_full, x_own, y_out, r_y, phases=None):
    ph = phases or {"prep", "norm", "kv", "q", "sb", "fox", "idx", "dsa", "merge", "ffn"}
    if "prep" in ph:
        wprep(c, lw["w_in"], "w_in", D, DIN)
        for i in range(3):
            wprep(c, lw["w_gate"][i], "w_gate", D, D, row0=i * D)
            wprep(c, lw["w_branch"][i], "w_branch", 1024, D, row0=i * 1024)
        wprep(c, lw["w_out"], "w_out", D, D)
        wprep(c, lw["w_ffn_gate"], "w_fg", D, DFF)
        wprep(c, lw["w_ffn_up"], "w_fu", D, DFF)
        wprep(c, lw["w_ffn_down"], "w_fd", DFF, D)
    if "norm" in ph:
        rmsnorm_T(c, x_full, lw["norm_mix_g"], c.S, c.hT, c.r_hT, "n1_")
        rmsnorm_T(c, x_own, lw["norm_mix_g"], c.SO, c.hTo, c.r_hTo, "n2_")
    if "kv" in ph:
        proj_phase(c, lw, own=False)
    if "q" in ph:
        proj_phase(c, lw, own=True)
    if "sb" in ph:
        attn_phase(c, "sb")
    if "fox" in ph:
        attn_phase(c, "fox")
    if "idx" in ph:
        indexer_phase(c)
    if "dsa" in ph:
        attn_phase(c, "dsa")
    if "merge" in ph:
        merge_phase(c, x_own)
        rmsnorm_T(c, c.x1, lw["norm_ffn_g"], c.SO, c.h2T, c.r_h2T, "n3_")
    if "ffn" in ph:
        ffn_phase(c, y_out, r_y)

import numpy as np
from concourse.bass_utils import run_bass_kernel_spmd

THETA = 10000.0
WNAMES = ["norm_mix_g", "w_in", "fox_f_bias", "fox_q_g", "fox_k_g", "dsa_q_g", "dsa_k_g", "w_gate", "w_branch", "w_out",
          "norm_ffn_g", "w_ffn_gate", "w_ffn_up", "w_ffn_down"]
WSHAPES = {"norm_mix_g": [D], "w_in": [D, DIN], "fox_f_bias": [8], "fox_q_g": [128], "fox_k_g": [128], "dsa_q_g": [128], "dsa_k_g": [128],
           "w_gate": [3, D, D], "w_branch": [3, 1024, D], "w_out": [D, D], "norm_ffn_g": [D], "w_ffn_gate": [D, DFF], "w_ffn_up": [D, DFF], "w_ffn_down": [DFF, D]}


def rope_np(S, dim):
    inv = (1.0 / (np.float32(THETA) ** (np.arange(0, dim, 2, dtype=np.float32) / np.float32(dim)))).astype(np.float32)
    ang = (np.arange(S, dtype=np.float32)[:, None] * inv[None, :]).astype(np.float32)
    return np.cos(ang).astype(np.float32), np.sin(ang).astype(np.float32)


def own_index(S, p):
    NT = S // 1024
    idx = np.concatenate([512 * (2 * m + p) + np.arange(512) for m in range(NT)])
    return idx


def make_consts(S, p):
    cs = {}
    cs["ident"] = np.eye(128, dtype=np.float32)
    cs["identf"] = np.eye(128, dtype=np.float32)
    j = np.arange(128)
    cs["trin"] = -(j[:, None] >= j[None, :]).astype(np.float32)
    cs["onesn"] = -np.ones((128, 128), np.float32)
    cs["ones"] = np.ones((128, 128), np.float32)
    cs["onesf"] = np.ones((128, 128), np.float32)
    rot = np.zeros((128, 128), np.float32)
    for d2 in range(128):
        if d2 < 64:
            rot[d2 + 64, d2] = -1.0
        else:
            rot[d2 - 64, d2] = 1.0
    cs["rot"] = rot
    roti = np.zeros((128, 128), np.float32)
    for blk in range(2):
        for d2 in range(64):
            if d2 < 32:
                roti[blk * 64 + d2 + 32, blk * 64 + d2] = -1.0
            else:
                roti[blk * 64 + d2 - 32, blk * 64 + d2] = 1.0
    cs["roti"] = roti
    key = (128 * np.arange(8)[:, None, None] + np.arange(128)[None, :, None])
    qry = 512 * p + np.arange(512)[None, None, :]
    cs["cm_strict"] = (key < qry).astype(np.float32)
    cs["cm_add"] = np.where(key <= qry, 0.0, NEG).astype(np.float32)
    q2 = 512 * p + 128 * np.arange(4)[:, None, None] + np.arange(128)[None, :, None]
    k2 = np.arange(1024)[None, None, :]
    cs["cmq_add"] = np.where(k2 <= q2, 0.0, NEG).astype(np.float32)
    sel = np.zeros((128, 2), np.float32); sel[:, p] = 1.0
    cs["sel"] = sel
    ch, sh = rope_np(S, 128)
    ci, si = rope_np(S, 64)
    cs["cos_h"] = np.ascontiguousarray(np.concatenate([ch, ch], 1).T)
    cs["sin_h"] = np.ascontiguousarray(np.concatenate([sh, sh], 1).T)
    cs["cos_i"] = np.ascontiguousarray(np.concatenate([ci, ci, ci, ci], 1).T)
    cs["sin_i"] = np.ascontiguousarray(np.concatenate([si, si, si, si], 1).T)
    oi = own_index(S, p)
    for n in ["cos_h", "sin_h", "cos_i", "sin_i"]:
        cs[n + "_own"] = np.ascontiguousarray(cs[n][:, oi])
    return cs


CONST_SHAPES = lambda S: {"ident": [128, 128], "identf": [128, 128], "trin": [128, 128], "onesn": [128, 128], "ones": [128, 128], "onesf": [128, 128],
                          "rot": [128, 128], "roti": [128, 128], "cm_strict": [8, 128, 512], "cm_add": [8, 128, 512], "cmq_add": [4, 128, 1024], "sel": [128, 2],
                          "cos_h": [128, S], "sin_h": [128, S], "cos_i": [128, S], "sin_i": [128, S],
                          "cos_h_own": [128, S // 2], "sin_h_own": [128, S // 2], "cos_i_own": [128, S // 2], "sin_i_own": [128, S // 2]}


def build_one_layer(S, phases=None, dbg=()):
    nc = bass.Bass("TRN2", target_bir_lowering=False)
    P = Prog(nc)
    cin = {n: nc.dram_tensor("c_" + n, shp, F32, kind="ExternalInput").ap() for n, shp in CONST_SHAPES(S).items()}
    lw = {n: nc.dram_tensor(n, WSHAPES[n], F32, kind="ExternalInput").ap() for n in WNAMES}
    x_full = nc.dram_tensor("x_full", [S, D], F32, kind="ExternalInput").ap()
    x_own = nc.dram_tensor("x_own", [S // 2, D], F32, kind="ExternalInput").ap()
    y = nc.dram_tensor("y", [S // 2, D], F32, kind="ExternalOutput").ap()
    r_y = P.res("y")
    c = make_ctx(nc, P, S)
    outs = [r_y]
    with contextlib.ExitStack() as es:
        load_consts(c, es, cin)
        build_layer(c, lw, x_full, x_own, y, r_y, phases)
        for name in dbg:
            src = {"hT": c.hT, "kT_sb": c.kT["sb"], "kT_fox": c.kT["fox"], "kT_dsa": c.kT["dsa"], "ikT": c.ikT, "v_sb": c.v["sb"], "v_dsa": c.v["dsa"],
                   "cT": c.cT, "qT_sb": c.qT["sb"], "qT_fox": c.qT["fox"], "qT_dsa": c.qT["dsa"], "iqT": c.iqT, "iw": c.iw, "oT": c.oT, "maskT": c.maskT,
                   "x1": c.x1, "h2T": c.h2T, "hTo": c.hTo}[name]
            rsrc = {"hT": c.r_hT, "hTo": c.r_hTo, "oT": c.r_oT, "maskT": c.r_maskT, "x1": c.r_x1, "h2T": c.r_h2T, "iw": c.r_q, "iqT": c.r_q,
                    "qT_sb": c.r_q, "qT_fox": c.r_q, "qT_dsa": c.r_q}.get(name, c.r_kv)
            shp = list(src.shape)
            o = nc.dram_tensor("dbg_" + name, shp, src.dtype, kind="ExternalOutput").ap()
            ro = P.res("dbg_" + name)
            nd = len(shp)
            if nd == 4:
                for i0 in range(shp[0]):
                    P.dma("sp", lambda e, o=o, src=src, i0=i0: e.dma_start(out=o[i0], in_=src[i0]), ro, reads=[rsrc], nowaw=True)
            else:
                P.dma("sp", lambda e, o=o, src=src: e.dma_start(out=o, in_=src), ro, reads=[rsrc], nowaw=True)
            outs.append(ro)
        P.finish(outs)
        P.emit()
    print("sems", P.n_sems, "ops", {e: len(P.ops[e]) for e in P.ENGS})
    return nc


def run_layer(nc, S, x, lws, dbg=()):
    in_maps = []
    for core in range(8):
        b, p = core // 2, core % 2
        cs = make_consts(S, p)
        m = {"c_" + k: np.ascontiguousarray(v, dtype=np.float32) for k, v in cs.items()}
        for n in WNAMES:
            m[n] = np.ascontiguousarray(lws[n], dtype=np.float32)
        m["x_full"] = np.ascontiguousarray(x[b])
        m["x_own"] = np.ascontiguousarray(x[b][own_index(S, p)])
        in_maps.append(m)
    res = run_bass_kernel_spmd(nc, in_maps, core_ids=list(range(8)))
    y = np.zeros_like(x)
    for core in range(8):
        b, p = core // 2, core % 2
        y[b][own_index(S, p)] = res.results[core]["y"]
    return y, res.results


_NC_CACHE = {}


def kernel(x, norm_mix_g, w_in, fox_f_bias, fox_q_g, fox_k_g, dsa_q_g, dsa_k_g, w_gate, w_branch, w_out,
           norm_ffn_g, w_ffn_gate, w_ffn_up, w_ffn_down):
    ws = dict(norm_mix_g=norm_mix_g, w_in=w_in, fox_f_bias=fox_f_bias, fox_q_g=fox_q_g, fox_k_g=fox_k_g, dsa_q_g=dsa_q_g,
              dsa_k_g=dsa_k_g, w_gate=w_gate, w_branch=w_branch, w_out=w_out, norm_ffn_g=norm_ffn_g,
              w_ffn_gate=w_ffn_gate, w_ffn_up=w_ffn_up, w_ffn_down=w_ffn_down)
    x = np.ascontiguousarray(np.asarray(x, dtype=np.float32))
    S = x.shape[1]
    depth = np.asarray(w_in).shape[0]
    if S not in _NC_CACHE:
        _NC_CACHE[S] = build_one_layer(S)
    nc = _NC_CACHE[S]
    cur = x
    for l in range(depth):
        lws = {n: np.asarray(ws[n])[l] for n in WNAMES}
        cur, _ = run_layer(nc, S, cur, lws)
    return cur.astype(np.float32)
```
